# Optimizing a Trainium2 kernel written in Bass

```python
import math
import jax, jax.numpy as jnp
from jax import lax
import numpy as np

D_MODEL = 1024
BATCH = 4
SEQ = 8192
DEPTH = 2

GRID_W = 64
MEM_LEN = 256
N_GROUPS = 4
GROUP_W = D_MODEL // N_GROUPS
FNET_GROUPS = 4
FNET_CH = GROUP_W // FNET_GROUPS
CONV_K = 31
GQA_HEAD_DIM = 64
GQA_Q_HEADS = GROUP_W // GQA_HEAD_DIM
GQA_KV_HEADS = 2
MLA_HEADS = 4
MLA_NOPE = 64
MLA_ROPE = 32
MLA_V = GROUP_W // MLA_HEADS
MLA_Q_RANK = 256
MLA_KV_RANK = 128
X_HEADS = 4
X_HEAD_DIM = D_MODEL // X_HEADS
N_EXPERTS = 32
TOP_K = 4
D_FF = D_MODEL
SWIGLU_LIMIT = 7.0
SWIGLU_ALPHA = 1.702
MOE_BLOCK = 256
Q_BLOCK = 128
ROPE_THETA = 10000.0
LN_EPS = 1e-5
RMS_EPS = 1e-6
DN_ALPHA = (2 * DEPTH) ** 0.25
DN_BETA = (8 * DEPTH) ** -0.25

F_COLS = GROUP_W
C_COLS = 2 * GROUP_W
GQ_Q_COLS = GQA_Q_HEADS * GQA_HEAD_DIM
GQ_KV_COLS = GQA_KV_HEADS * GQA_HEAD_DIM
IN_COLS = F_COLS + C_COLS + GQ_Q_COLS + 2 * GQ_KV_COLS + MLA_Q_RANK + MLA_KV_RANK + MLA_ROPE
SPLITS = (F_COLS,
          F_COLS + C_COLS,
          F_COLS + C_COLS + GQ_Q_COLS,
          F_COLS + C_COLS + GQ_Q_COLS + GQ_KV_COLS,
          F_COLS + C_COLS + GQ_Q_COLS + 2 * GQ_KV_COLS,
          F_COLS + C_COLS + GQ_Q_COLS + 2 * GQ_KV_COLS + MLA_Q_RANK,
          F_COLS + C_COLS + GQ_Q_COLS + 2 * GQ_KV_COLS + MLA_Q_RANK + MLA_KV_RANK)

kernel_name = "hybrid_parallel_group_encoder"


def layer_norm(x, g, b):
    xf = x.astype(jnp.float32)
    mu = jnp.mean(xf, axis=-1, keepdims=True)
    var = jnp.mean(jnp.square(xf - mu), axis=-1, keepdims=True)
    return ((xf - mu) * lax.rsqrt(var + LN_EPS)).astype(x.dtype) * g + b


def rms_norm(x, g):
    xf = x.astype(jnp.float32)
    r = lax.rsqrt(jnp.mean(jnp.square(xf), axis=-1, keepdims=True) + RMS_EPS)
    return (xf * r).astype(x.dtype) * g


def rope_tables(pos, dim):
    inv = ROPE_THETA ** (-jnp.arange(0, dim, 2, dtype=jnp.float32) / dim)
    ang = pos.astype(jnp.float32)[:, None] * inv[None, :]
    return (jnp.cos(ang), jnp.sin(ang))


def apply_rope(x, cos, sin):
    half = x.shape[-1] // 2
    c = cos[:, None, :].astype(x.dtype)
    s = sin[:, None, :].astype(x.dtype)
    x1, x2 = x[..., :half], x[..., half:]
    return jnp.concatenate([x1 * c - x2 * s, x2 * c + x1 * s], axis=-1)


def axial_rope(x, tabs):
    cos_r, sin_r, cos_c, sin_c = tabs
    half = x.shape[-1] // 2
    return jnp.concatenate([apply_rope(x[..., :half], cos_r, sin_r),
                            apply_rope(x[..., half:], cos_c, sin_c)], axis=-1)


def blocked_attention(q, k, v, scale):
    B, S, Hq, dk = q.shape
    Hkv = k.shape[2]
    G = Hq // Hkv
    dv = v.shape[-1]
    nb = S // Q_BLOCK
    qb = q.reshape(B, nb, Q_BLOCK, Hkv, G, dk).transpose(1, 0, 2, 3, 4, 5)

    def one_block(qblk):
        s = jnp.einsum('bqhgd,bkhd->bhgqk', qblk, k).astype(jnp.float32) * scale
        p = jax.nn.softmax(s, axis=-1).astype(v.dtype)
        return jnp.einsum('bhgqk,bkhd->bqhgd', p, v)

    o = lax.map(one_block, qb)
    return o.transpose(1, 0, 2, 3, 4, 5).reshape(B, S, Hq * dv)


def hybrid_mixer(h, rope_g, rope_m, w_in, w_f, b_f, dw_w, dw_b, cg, cb, w_pw, b_pw,
                 qg, kg, mqg, w_uq, mkg, w_ukv, grp_g, w_o):
    B, S, _ = h.shape
    z = h @ w_in
    zf, zc, zq, zk, zv, zcq, zckv, zkr = jnp.split(z, SPLITS, axis=-1)

    u = zf.reshape(B, S, FNET_GROUPS, FNET_CH).astype(jnp.float32)
    yf = jnp.fft.fft2(u, axes=(1, 3)).real.astype(h.dtype).reshape(B, S, GROUP_W) @ w_f + b_f

    a, g = jnp.split(zc, 2, axis=-1)
    u = a * jax.nn.sigmoid(g)
    u = lax.conv_general_dilated(u, dw_w[:, None, :], (1,), [(CONV_K // 2, CONV_K // 2)],
                                 dimension_numbers=('NWC', 'WIO', 'NWC'),
                                 feature_group_count=GROUP_W) + dw_b
    yc = jax.nn.silu(layer_norm(u, cg, cb)) @ w_pw + b_pw

    q = rms_norm(zq.reshape(B, S, GQA_Q_HEADS, GQA_HEAD_DIM), qg)
    k = rms_norm(zk.reshape(B, S, GQA_KV_HEADS, GQA_HEAD_DIM), kg)
    v = zv.reshape(B, S, GQA_KV_HEADS, GQA_HEAD_DIM)
    yg = blocked_attention(axial_rope(q, rope_g), axial_rope(k, rope_g), v, GQA_HEAD_DIM ** -0.5)

    qm = (rms_norm(zcq, mqg) @ w_uq).reshape(B, S, MLA_HEADS, MLA_NOPE + MLA_ROPE)
    kvm = (rms_norm(zckv, mkg) @ w_ukv).reshape(B, S, MLA_HEADS, MLA_NOPE + MLA_V)
    q_nope, q_rope = qm[..., :MLA_NOPE], qm[..., MLA_NOPE:]
    k_nope, vm = kvm[..., :MLA_NOPE], kvm[..., MLA_NOPE:]
    k_rope = axial_rope(zkr[:, :, None, :], rope_m)
    qm = jnp.concatenate([q_nope, axial_rope(q_rope, rope_m)], axis=-1)
    km = jnp.concatenate([k_nope, jnp.broadcast_to(k_rope, (B, S, MLA_HEADS, MLA_ROPE))], axis=-1)
    ym = blocked_attention(qm, km, vm, (MLA_NOPE + MLA_ROPE) ** -0.5)

    y = rms_norm(jnp.stack([yf, yc, yg, ym], axis=2), grp_g).reshape(B, S, D_MODEL)
    return y @ w_o


def memory_cross_attention(h, mem, w_xq, w_xk, w_xv, w_xo):
    B, S, _ = h.shape
    M = mem.shape[1]
    q = (h @ w_xq).reshape(B, S, X_HEADS, X_HEAD_DIM)
    k = (mem @ w_xk).reshape(B, M, X_HEADS, X_HEAD_DIM)
    v = (mem @ w_xv).reshape(B, M, X_HEADS, X_HEAD_DIM)
    return blocked_attention(q, k, v, X_HEAD_DIM ** -0.5) @ w_xo


def routed_experts(h, w_router, b_router, w_gu, b_gu, w_down, b_down):
    B, S, D = h.shape
    T = B * S
    xt = h.reshape(T, D)
    logits = (xt @ w_router + b_router).astype(jnp.float32)
    top_v, top_i = lax.top_k(logits, TOP_K)
    gates = jax.nn.softmax(top_v, axis=-1).astype(h.dtype)

    N = T * TOP_K
    flat_e = top_i.reshape(N)
    order = jnp.argsort(flat_e)
    sorted_e = flat_e[order]
    counts = jnp.bincount(flat_e, length=N_EXPERTS)
    padded = ((counts + MOE_BLOCK - 1) // MOE_BLOCK) * MOE_BLOCK
    start = jnp.cumsum(counts) - counts
    pend = jnp.cumsum(padded)
    pstart = pend - padded
    dest = pstart[sorted_e] + (jnp.arange(N) - start[sorted_e])
    n_blocks = -(-N // MOE_BLOCK) + N_EXPERTS
    P = n_blocks * MOE_BLOCK
    slot_tok = jnp.zeros((P,), jnp.int32).at[dest].set((order // TOP_K).astype(jnp.int32))
    slot_w = jnp.zeros((P,), h.dtype).at[dest].set(gates.reshape(N)[order])
    block_e = jnp.minimum(jnp.searchsorted(pend, jnp.arange(n_blocks) * MOE_BLOCK, side='right'),
                          N_EXPERTS - 1)
    xs = xt[slot_tok].reshape(n_blocks, MOE_BLOCK, D)

    def expert_block(args):
        xb, e = args
        gu = xb @ w_gu[e] + b_gu[e]
        gate, up = gu[:, :D_FF], gu[:, D_FF:]
        gate = jnp.minimum(gate, SWIGLU_LIMIT)
        up = jnp.clip(up, -SWIGLU_LIMIT, SWIGLU_LIMIT)
        act = (up + 1.0) * (gate * jax.nn.sigmoid(SWIGLU_ALPHA * gate))
        return act @ w_down[e] + b_down[e]

    ys = lax.map(expert_block, (xs, block_e)).reshape(P, D)
    out = jnp.zeros((T, D), h.dtype).at[slot_tok].add(ys * slot_w[:, None])
    return out.reshape(B, S, D)


def setup_inputs(seed: int = 0) -> dict:
    key = jax.random.key(seed)
    ks = iter(jax.random.split(key, 48))
    L, D, E, F, G = DEPTH, D_MODEL, N_EXPERTS, D_FF, GROUP_W

    def nrm(shape, scale):
        return jax.random.normal(next(ks), shape, jnp.float32) * scale

    def gain(shape):
        return 1.0 + nrm(shape, 0.02)

    return {
        "x": nrm((BATCH, SEQ, D), 1.0),
        "mem": nrm((BATCH, MEM_LEN, D), 1.0),
        "ln_in_g": gain((D,)),
        "ln_in_b": nrm((D,), 0.02),
        "w_in": nrm((L, D, IN_COLS), D ** -0.5),
        "w_f": nrm((L, G, G), G ** -0.5),
        "b_f": nrm((L, G), 0.02),
        "dw_w": nrm((L, CONV_K, G), CONV_K ** -0.5),
        "dw_b": nrm((L, G), 0.02),
        "conv_ln_g": gain((L, G)),
        "conv_ln_b": nrm((L, G), 0.02),
        "w_pw": nrm((L, G, G), G ** -0.5),
        "b_pw": nrm((L, G), 0.02),
        "q_norm_g": gain((L, GQA_HEAD_DIM)),
        "k_norm_g": gain((L, GQA_HEAD_DIM)),
        "mla_q_norm_g": gain((L, MLA_Q_RANK)),
        "w_uq": nrm((L, MLA_Q_RANK, MLA_HEADS * (MLA_NOPE + MLA_ROPE)), MLA_Q_RANK ** -0.5),
        "mla_kv_norm_g": gain((L, MLA_KV_RANK)),
        "w_ukv": nrm((L, MLA_KV_RANK, MLA_HEADS * (MLA_NOPE + MLA_V)), MLA_KV_RANK ** -0.5),
        "grp_norm_g": gain((L, N_GROUPS, G)),
        "w_o": nrm((L, D, D), D ** -0.5 * DN_BETA),
        "ln1_g": gain((L, D)),
        "ln1_b": nrm((L, D), 0.02),
        "w_xq": nrm((L, D, D), D ** -0.5),
        "w_xk": nrm((L, D, D), D ** -0.5),
        "w_xv": nrm((L, D, D), D ** -0.5),
        "w_xo": nrm((L, D, D), D ** -0.5 * DN_BETA),
        "ln2_g": gain((L, D)),
        "ln2_b": nrm((L, D), 0.02),
        "w_router": nrm((L, D, E), D ** -0.5),
        "b_router": nrm((L, E), 0.01),
        "w_gu": nrm((L, E, D, 2 * F), D ** -0.5),
        "b_gu": nrm((L, E, 2 * F), 0.02),
        "w_down": nrm((L, E, F, D), F ** -0.5 * DN_BETA),
        "b_down": nrm((L, E, D), 0.02),
        "ln3_g": gain((L, D)),
        "ln3_b": nrm((L, D), 0.02),
    }


def reference(x, mem, ln_in_g, ln_in_b, w_in, w_f, b_f, dw_w, dw_b, conv_ln_g, conv_ln_b,
              w_pw, b_pw, q_norm_g, k_norm_g, mla_q_norm_g, w_uq, mla_kv_norm_g, w_ukv,
              grp_norm_g, w_o, ln1_g, ln1_b, w_xq, w_xk, w_xv, w_xo, ln2_g, ln2_b,
              w_router, b_router, w_gu, b_gu, w_down, b_down, ln3_g, ln3_b):
    S = x.shape[1]
    ROWS = S // GRID_W
    row = jnp.repeat(jnp.arange(ROWS, dtype=jnp.int32), GRID_W)
    col = jnp.tile(jnp.arange(GRID_W, dtype=jnp.int32), ROWS)
    rope_g = rope_tables(row, GQA_HEAD_DIM // 2) + rope_tables(col, GQA_HEAD_DIM // 2)
    rope_m = rope_tables(row, MLA_ROPE // 2) + rope_tables(col, MLA_ROPE // 2)

    h = layer_norm(x, ln_in_g, ln_in_b)
    for l in range(DEPTH):
        mix = hybrid_mixer(h, rope_g, rope_m, w_in[l], w_f[l], b_f[l], dw_w[l], dw_b[l],
                           conv_ln_g[l], conv_ln_b[l], w_pw[l], b_pw[l], q_norm_g[l], k_norm_g[l],
                           mla_q_norm_g[l], w_uq[l], mla_kv_norm_g[l], w_ukv[l], grp_norm_g[l], w_o[l])
        h = layer_norm(DN_ALPHA * h + mix, ln1_g[l], ln1_b[l])
        xa = memory_cross_attention(h, mem, w_xq[l], w_xk[l], w_xv[l], w_xo[l])
        h = layer_norm(DN_ALPHA * h + xa, ln2_g[l], ln2_b[l])
        ff = routed_experts(h, w_router[l], b_router[l], w_gu[l], b_gu[l], w_down[l], b_down[l])
        h = layer_norm(DN_ALPHA * h + ff, ln3_g[l], ln3_b[l])
    return h
```

```python
import numpy as np
import concourse.bass as bass
import concourse.mybir as mybir

F32 = mybir.dt.float32
BF16 = mybir.dt.bfloat16
I32 = mybir.dt.int32
ALU = mybir.AluOpType
AF = mybir.ActivationFunctionType
AX = mybir.AxisListType

ENGS = ("pe", "act", "dve", "pool", "sp")
EPOCH = 8000
STRICT = True


class Buf:
    __slots__ = ("name", "lw", "rd", "dsem", "dcnt", "is_dram", "excl")

    def __init__(self, name, is_dram=False):
        self.name = name
        self.excl = False
        self.lw = {}
        self.rd = {}
        self.dsem = None
        self.dcnt = 0
        self.is_dram = is_dram


class T:
    __slots__ = ("buf", "ap")

    def __init__(self, buf, ap):
        self.buf = buf
        self.ap = ap

    def __getitem__(self, idx):
        return T(self.buf, self.ap[idx])

    def re(self, pat, **kw):
        return T(self.buf, self.ap.rearrange(pat, **kw))

    def bitcast(self, dt):
        return T(self.buf, self.ap.bitcast(dt))

    def bc(self, axis, shape):
        return T(self.buf, self.ap.unsqueeze(axis).broadcast_to(list(shape)))

    @property
    def shape(self):
        return self.ap.shape


class Prog:
    def __init__(self, nc):
        self.nc = nc
        self.ops = {e: [] for e in ENGS}
        self.seen = {e: {} for e in ENGS}
        self.dsems = []
        self.nbuf = 0
        self.dlast = {}
        self.ddsem = None
        self.free_dsems = []
        self.cur_bufs = None

    def begin_phase(self):
        self.cur_bufs = []
        self.phase_id = getattr(self, "phase_id", 0) + 1

    def end_phase(self):
        self.barrier()
        for b in self.cur_bufs:
            self.free_dsems.append((b.dsem, b.dcnt))
            b.dsem = None
        self.cur_bufs = None

    def sb(self, name, shape, dt, n=1):
        name = "g_" + name
        if n == 1:
            t = self.nc.alloc_sbuf_tensor(name, list(shape), dt)
            return T(Buf(name), t.ap())
        t = self.nc.alloc_sbuf_tensor(name, [shape[0], n] + list(shape[1:]), dt)
        a = t.ap()
        return [T(Buf(f"{name}{i}"), a[:, i]) for i in range(n)]

    def ps(self, name, shape, dt=F32, n=1):
        out = []
        for i in range(n):
            t = self.nc.alloc_psum_tensor(f"{name}{i}", list(shape), dt)
            b = Buf(f"{name}{i}")
            b.excl = True
            out.append(T(b, t.ap()))
        return out[0] if n == 1 else out

    def dram(self, name, shape, dt, kind="Internal"):
        t = self.nc.dram_tensor(name, list(shape), dt, kind=kind)
        return T(Buf(name, is_dram=True), t.ap())

    def view(self, t, name):
        return T(Buf(name), t.ap)

    def _need(self, eng, events, waits):
        seen = self.seen[eng]
        for key, val in events.items():
            if key == eng and (not STRICT or eng in ("pe", "sp")):
                continue
            if seen.get(key, -1) >= val:
                continue
            seen[key] = val
            waits.append((key, val))
            if not isinstance(key, tuple):
                self.ops[key][val]["sig"] = True

    def op(self, eng, emit, r=(), w=()):
        waits = []
        for t in r:
            self._need(eng, t.buf.lw, waits)
            if t.buf.excl:
                self._need(eng, {k: v for k, v in t.buf.rd.items() if k != eng}, waits)
        for t in w:
            self._need(eng, t.buf.lw, waits)
            self._need(eng, t.buf.rd, waits)
        idx = len(self.ops[eng])
        self.ops[eng].append(dict(emit=emit, waits=waits, sig=False, dma=None))
        for t in w:
            t.buf.lw = {eng: idx}
            t.buf.rd = {}
        for t in r:
            if t.buf in [x.buf for x in w]:
                continue
            t.buf.rd[eng] = idx
        return idx

    def call(self, eng, meth, r=(), w=(), **kw):
        rr = list(r)
        ww = list(w)
        kk = {}
        for k, v in kw.items():
            if isinstance(v, T):
                (ww if k in ("out", "accum_out") else rr).append(v)
                kk[k] = v.ap
            else:
                kk[k] = v

        def emit(e, meth=meth, kk=kk):
            return getattr(e, meth)(**kk)
        return self.op(eng, emit, rr, ww)

    def dma(self, q, out, in_, **kw):
        sbt = out if not out.buf.is_dram else in_
        assert not sbt.buf.is_dram
        b = sbt.buf
        if b.dsem is None:
            if self.free_dsems:
                b.dsem, b.dcnt = self.free_dsems.pop()
            else:
                b.dsem = len(self.dsems)
                self.dsems.append(self.nc.alloc_semaphore(f"d{b.dsem}"))
            if self.cur_bufs is not None:
                self.cur_bufs.append(b)
        key = ("d", b.dsem)
        waits = []
        if b.dcnt > 0:
            self._need(q, {key: 16 * b.dcnt}, waits)
        self._need(q, in_.buf.lw, waits)
        if not out.buf.is_dram:
            self._need(q, out.buf.lw, waits)
        self._need(q, out.buf.rd, waits)
        b.dcnt += 1
        val = 16 * b.dcnt
        oa, ia = out.ap, in_.ap
        sem = self.dsems[b.dsem]

        def emit(e, oa=oa, ia=ia, kw=kw, sem=sem):
            return e.dma_start(out=oa, in_=ia, **kw).then_inc(sem, 16)
        self.ops[q].append(dict(emit=emit, waits=waits, sig=False, dma=True))
        if out.buf.is_dram:
            out.buf.lw[key] = val
        else:
            out.buf.lw = {key: val}
        if not out.buf.is_dram:
            out.buf.rd = {}
        if in_.buf is not out.buf:
            in_.buf.rd[key] = val
        self.dlast[key] = val

    def cc(self, q, emitfn, src, dst):
        idx = len(self.dsems)
        sem = self.nc.alloc_semaphore(f"cc{idx}")
        self.dsems.append(sem)
        key = ("d", idx)
        waits = []
        self._need(q, src.buf.lw, waits)
        self._need(q, dst.buf.lw, waits)
        self._need(q, dst.buf.rd, waits)

        def emit(e, emitfn=emitfn, sem=sem):
            return emitfn(e).then_inc(sem, 1)
        self.ops[q].append(dict(emit=emit, waits=waits, sig=False, dma=True))
        dst.buf.lw[key] = 1
        src.buf.rd[key] = 1
        self.dlast[key] = 1

    def dma_dd(self, q, out, in_, **kw):
        if self.ddsem is None:
            self.ddsem = len(self.dsems)
            self.dsems.append(self.nc.alloc_semaphore("ddsem"))
            self.ddcnt = 0
        key = ("d", self.ddsem)
        waits = []
        if self.ddcnt > 0:
            self._need(q, {key: 16 * self.ddcnt}, waits)
        self._need(q, in_.buf.lw, waits)
        self._need(q, out.buf.rd, waits)
        self.ddcnt += 1
        val = 16 * self.ddcnt
        oa, ia = out.ap, in_.ap
        sem = self.dsems[self.ddsem]

        def emit(e, oa=oa, ia=ia, kw=kw, sem=sem):
            return e.dma_start(out=oa, in_=ia, **kw).then_inc(sem, 16)
        self.ops[q].append(dict(emit=emit, waits=waits, sig=False, dma=True))
        out.buf.lw[key] = val
        in_.buf.rd[key] = val
        self.dlast[key] = val

    def barrier(self):
        ev = dict(self.dlast)
        for e in ENGS:
            for i in range(len(self.ops[e]) - 1, -1, -1):
                if self.ops[e][i]["dma"] is None and self.ops[e][i]["emit"] is not None:
                    ev[e] = i
                    break
        for e in ENGS:
            waits = []
            saved = STRICT
            self._need(e, {k: v for k, v in ev.items() if k != e}, waits)
            self.ops[e].append(dict(emit=None, waits=waits, sig=False, dma=None))

    def wait_all_dma(self, eng):
        waits = []
        for i, s in enumerate(self.dsems):
            pass
        return waits

    def final_wait(self, eng, bufs):
        waits = []
        for t in bufs:
            self._need(eng, t.buf.lw, waits)
        self.ops[eng].append(dict(emit=None, waits=waits, sig=False, dma=None))

    def build(self):
        nc = self.nc
        esems = {}
        sigcnt = {}
        for e in ENGS:
            n = 0
            cnt = []
            for o in self.ops[e]:
                if o["sig"]:
                    n += 1
                cnt.append(n)
            sigcnt[e] = cnt
            nep = (n + EPOCH - 1) // EPOCH
            esems[e] = [nc.alloc_semaphore(f"e_{e}{i}") for i in range(max(nep, 1))]
        self.nsig = {e: (sigcnt[e][-1] if sigcnt[e] else 0) for e in ENGS}

        def resolve(key, val):
            if isinstance(key, tuple):
                return self.dsems[key[1]], val
            c = sigcnt[key][val]
            ep = (c - 1) // EPOCH
            return esems[key][ep], c - ep * EPOCH

        engobj = {"pe": "tensor", "act": "scalar", "dve": "vector", "pool": "gpsimd", "sp": "sync"}
        with nc.Block() as block:
            for e in ENGS:
                ops = self.ops[e]

                def body(eng, e=e, ops=ops):
                    for i, o in enumerate(ops):
                        for key, val in o["waits"]:
                            s, v = resolve(key, val)
                            eng.wait_ge(s, v)
                        if o["emit"] is None:
                            continue
                        ins = o["emit"](eng)
                        if o["sig"]:
                            c = sigcnt[e][i]
                            ep = (c - 1) // EPOCH
                            ins.then_inc(esems[e][ep], 1)
                getattr(block, engobj[e])(body)
        return nc

from concourse.bass_utils import run_bass_kernel_spmd

import math
from contextlib import ExitStack

D = 1024
NCOL = 1696
NE = 32
ALPHA = 4.0 ** 0.25
LN_EPS = 1e-5
RMS_EPS = 1e-6
MEM = 256


class KB:
    pass


K_LAST = [None]


def build_program(cfg):
    S = cfg["S"]
    TT = S // 2
    L = cfg["L"]
    nc = bass.Bass("TRN2", target_bir_lowering=False)
    P = Prog(nc)
    K = KB()
    K.S, K.TT, K.L, K.cfg = S, TT, L, cfg
    K.P = P
    dbg = cfg.get("dbg", ())

    def din(name, shape, dt=F32):
        return P.dram(name, shape, dt, kind="ExternalInput")

    I = {}
    I["xown"] = din("xown", [TT, D])
    I["mem"] = din("mem", [MEM, D])
    I["ln_in_g"] = din("ln_in_g", [D])
    I["ln_in_b"] = din("ln_in_b", [D])
    I["w_in"] = din("w_in", [L, D, NCOL])
    I["w_f"] = din("w_f", [L, 256, 256])
    I["b_f"] = din("b_f", [L, 256])
    I["dw_w"] = din("dw_w", [L, 31, 256])
    I["dw_b"] = din("dw_b", [L, 256])
    I["conv_ln_g"] = din("conv_ln_g", [L, 256])
    I["conv_ln_b"] = din("conv_ln_b", [L, 256])
    I["w_pw"] = din("w_pw", [L, 256, 256])
    I["b_pw"] = din("b_pw", [L, 256])
    I["q_norm_g"] = din("q_norm_g", [L, 64])
    I["k_norm_g"] = din("k_norm_g", [L, 64])
    I["mla_q_norm_g"] = din("mla_q_norm_g", [L, 256])
    I["w_uq"] = din("w_uq", [L, 256, 384])
    I["mla_kv_norm_g"] = din("mla_kv_norm_g", [L, 128])
    I["w_ukv"] = din("w_ukv", [L, 128, 512])
    I["grp_norm_g"] = din("grp_norm_g", [L, 1024])
    I["w_o"] = din("w_o", [L, D, D])
    for n in ("ln1_g", "ln1_b", "ln2_g", "ln2_b", "ln3_g", "ln3_b"):
        I[n] = din(n, [L, D])
    for n in ("w_xq", "w_xk", "w_xv", "w_xo"):
        I[n] = din(n, [L, D, D])
    I["w_router"] = din("w_router", [L, D, NE])
    I["b_router"] = din("b_router", [L, NE])
    NEL = NE // 8
    if cfg.get("moe", True):
        I["w_gu_sh"] = din("w_gu_sh", [L, NEL * D, 2 * D])
        I["w_down_sh"] = din("w_down_sh", [L, NEL * D, D])
    I["b_gu"] = din("b_gu", [L, NE, 2 * D])
    I["b_down"] = din("b_down", [L, NE, D])
    I["ident"] = din("ident", [128, 128])
    I["ropefull"] = din("ropefull", [S, 192])
    I["ropeown"] = din("ropeown", [TT, 192])
    I["dftc_sh"] = din("dftc_sh", [S // 8, TT], BF16)
    I["dfts_sh"] = din("dfts_sh", [S // 8, TT], BF16)
    I["bc"] = din("bc", [256, 256])
    I["bsn"] = din("bsn", [256, 256])
    I["cmask"] = din("cmask", [128, 7])
    K.I = I
    out = P.dram("out", [TT, D], F32, kind="ExternalOutput")

    K.dbg_out = {}

    def scr(name, shape, dt):
        if name in dbg:
            t = P.dram(name, shape, dt, kind="ExternalOutput")
            K.dbg_out[name] = t
            return t
        return P.dram(name, shape, dt)
    K.hown = P.dram("hown", [TT, D], F32)
    K.hnext = P.dram("hnext", [TT, D], F32)
    K.hfull = P.dram("hfull", [8 * TT, D], F32)
    K.Zd = scr("Zd", [S, 512], BF16)
    K.KTg = scr("KTg", [128, S], BF16)
    K.Vg = scr("Vg", [S, 2 * 65], BF16)
    K.QTg = scr("QTg", [256, TT], BF16)
    K.KTm = scr("KTm", [256, S], BF16)
    K.KRm = scr("KRm", [32, S], BF16)
    K.Vm = scr("Vm", [S, 4 * 65], BF16)
    K.QTm = scr("QTm", [4 * 96, TT], BF16)
    K.uT = scr("uT", [256, TT + 32], F32)
    K.Y = scr("Y", [TT, D], F32)
    if cfg.get("moe", True):
        K.wgu = [P.dram(f"wgu{l}", [NE * D, 2 * D], F32) for l in range(L)]
        K.wdn = [P.dram(f"wdn{l}", [NE * D, D], F32) for l in range(L)]
        K.wgu_in = [P.dram(f"wgui{l}", [NEL * D, 2 * D], F32) for l in range(L)]
        K.wdn_in = [P.dram(f"wdni{l}", [NEL * D, D], F32) for l in range(L)]
        K.wgub = P.dram("wgub", [NE * D, 2 * D], BF16)
        K.wdnb = P.dram("wdnb", [NE * D, D], BF16)

    K.ptr = P.ps("ptr", [128, 8, 128], BF16, n=2)
    K.pmm = P.ps("pmm", [128, 512], F32, n=4)
    K.pacc = P.ps("pacc", [128, 512], F32, n=2)
    K.pi = 0
    K.ti = 0
    K.idb = P.sb("idb", [128, 128], BF16)
    K.idf = P.sb("idf", [128, 128], F32)
    K.eps_ln = P.sb("eps_ln", [128, 1], F32)
    K.eps_rms = P.sb("eps_rms", [128, 1], F32)
    K.ones_b = P.sb("ones_b", [128, 128], BF16)
    K.ones_f = P.sb("ones_f", [128, 128], F32)
    K.cmask = P.sb("cmask_t", [128, 7], F32)
    P.K = K
    K.wstage = None
    K.wsi = 0
    P.dma("sp", K.idf, I["ident"])
    P.call("dve", "tensor_copy", out=K.idb, in_=K.idf)
    P.dma("sp", K.cmask, I["cmask"])
    P.call("dve", "memset", w=[K.eps_ln], ap=K.eps_ln.ap, constant=LN_EPS)
    P.call("dve", "memset", w=[K.eps_rms], ap=K.eps_rms.ap, constant=RMS_EPS)
    P.call("dve", "memset", w=[K.ones_b], ap=K.ones_b.ap, constant=1.0)
    P.call("dve", "memset", w=[K.ones_f], ap=K.ones_f.ap, constant=1.0)

    dft_i = P.dram("dft_i", [S // 8, 2 * TT], BF16)
    dft_f = P.dram("dft_f", [S, 2 * TT], BF16)
    P.dma_dd("sp", dft_i[:, 0:TT], I["dftc_sh"])
    P.dma_dd("sp", dft_i[:, TT:2 * TT], I["dfts_sh"])

    def ccd(e, i=dft_i.ap, o=dft_f.ap):
        return e.collective_compute("AllGather", ALU.bypass, replica_groups=[list(range(8))], ins=[i], outs=[o])
    P.cc("pool", ccd, dft_i, dft_f)
    K.dft = [dft_f[:, 0:TT], dft_f[:, TT:2 * TT]]
    if cfg.get("moe", True):
        for l in range(L):
            P.dma_dd("sp", K.wgu_in[l], I["w_gu_sh"][l])
            P.dma_dd("sp", K.wdn_in[l], I["w_down_sh"][l])
        for l in range(L):
            for (src, dst) in ((K.wgu_in[l], K.wgu[l]), (K.wdn_in[l], K.wdn[l])):
                def ccf(e, i=src.ap, o=dst.ap):
                    return e.collective_compute("AllGather", ALU.bypass, replica_groups=[list(range(8))], ins=[i], outs=[o])
                P.cc("pool", ccf, src, dst)

    phases = cfg.get("phases", ("A", "fnet", "conv", "attn", "wo", "xattn", "moe"))
    phase_ln_in(K)
    for l in range(L):
        K.l = l
        phase_gather(K)
        if "A" in phases:
            phase_A(K)
        if "fnet" in phases:
            phase_fnet(K)
        if "conv" in phases:
            phase_conv(K)
        if "attn" in phases:
            phase_attn(K)
        if "wo" in phases:
            phase_wo(K)
        if "xattn" in phases:
            phase_xattn(K)
        if "moe" in phases:
            phase_moe(K)
    phase_out(K, out)
    for name in dbg:
        pass
    P.final_wait("sp", [out] + list(K.dbg_out.values()))
    K_LAST[0] = P
    return P.build()


def next_pmm(K):
    t = K.pmm[K.pi % len(K.pmm)]
    K.pi += 1
    return t


def next_ptr(K):
    t = K.ptr[K.ti % 2]
    K.ti += 1
    return t


def bcast_row(P, K, st, name, src_ap_T, n, dt=F32, q="sp"):
    t = P.sb(name, [128, n], dt) if st is None else sbt(P, st, name, [128, n], dt)
    P.dma(q, t, T(src_ap_T.buf, src_ap_T.ap.partition_broadcast(128)))
    return t


_UID = [0]


def sbt(P, st, name, shape, dt, n=1):
    _UID[0] += 1
    name = f"s{_UID[0]}_{name}"
    if n == 1:
        t = st.enter_context(P.nc.sbuf_tensor(name, list(shape), dt))
        return T(Buf(name), t.ap())
    t = st.enter_context(P.nc.sbuf_tensor(name, [shape[0], n] + list(shape[1:]), dt))
    a = t.ap()
    return [T(Buf(f"{name}{i}"), a[:, i]) for i in range(n)]


def load_w_bf(P, st, name, wsrc, kchunks, ncols, q="sp"):
    K = P.K
    if K.wstage is None or K.wstage_phase != P.phase_id:
        K.wstage = sbt(P, st, "wstage", [128, 2048], F32, n=2)
        K.wstage_phase = P.phase_id
    t = sbt(P, st, name, [128, kchunks, ncols], BF16)
    for k in range(kchunks):
        c0 = 0
        while c0 < ncols:
            c1 = min(ncols, c0 + 2048)
            sg_ = K.wstage[K.wsi % 2]
            K.wsi += 1
            P.dma(q, sg_[:, 0:c1 - c0], wsrc[k * 128:(k + 1) * 128, c0:c1])
            if K.wsi % 2:
                P.call("act", "activation", out=t[:, k, c0:c1], in_=sg_[:, 0:c1 - c0], func=AF.Copy)
            else:
                P.call("dve", "tensor_copy", out=t[:, k, c0:c1], in_=sg_[:, 0:c1 - c0])
            c0 = c1
    return t


def ln_tile(P, K, A, src, dst, g_t, b_t, s):
    for c in range(2):
        P.call("dve", "bn_stats", out=A.st[s][:, c * 6:(c + 1) * 6], in_=src[:, c * 512:(c + 1) * 512])
    P.call("dve", "bn_aggr", out=A.mv[s], in_=A.st[s])
    P.call("act", "activation", out=A.rs[s], in_=A.mv[s][:, 1:2], func=AF.Sqrt, bias=K.eps_ln, scale=1.0)
    P.call("dve", "reciprocal", out=A.rs[s], in_=A.rs[s])
    P.call("dve", "tensor_scalar", out=dst, in0=src, scalar1=A.mv[s][:, 0:1], scalar2=A.rs[s], op0=ALU.subtract, op1=ALU.mult)
    P.call("pool", "tensor_tensor", out=dst, in0=dst, in1=g_t, op=ALU.mult)
    P.call("pool", "tensor_tensor", out=dst, in0=dst, in1=b_t, op=ALU.add)


def ln_scratch(P, st, pfx):
    A = KB()
    A.st = sbt(P, st, pfx + "st", [128, 12], F32, n=2)
    A.mv = sbt(P, st, pfx + "mv", [128, 2], F32, n=2)
    A.rs = sbt(P, st, pfx + "rs", [128, 1], F32, n=2)
    return A


def transpose_to(P, K, src_bf, dstT, nch, evac="dve"):
    pt = next_ptr(K)
    for k in range(nch):
        P.call("pe", "transpose", out=pt[:, k, :], in_=src_bf[:, k * 128:(k + 1) * 128], identity=K.idb)
    if evac == "dve":
        P.call("dve", "tensor_copy", out=dstT, in_=pt[:, 0:nch, :])
    else:
        P.call("act", "activation", out=dstT, in_=pt[:, 0:nch, :], func=AF.Copy)


def phase_ln_in(K):
    P, I, TT = K.P, K.I, K.TT
    P.begin_phase()
    with ExitStack() as st:
        g_t = bcast_row(P, K, st, "lng", I["ln_in_g"], D)
        b_t = bcast_row(P, K, st, "lnb", I["ln_in_b"], D)
        A = ln_scratch(P, st, "l0")
        xt = sbt(P, st, "l0x", [128, D], F32, n=2)
        ht = sbt(P, st, "l0h", [128, D], F32, n=2)
        for i in range(TT // 128):
            s = i % 2
            P.dma("sp", xt[s], I["xown"][i * 128:(i + 1) * 128, :])
            ln_tile(P, K, A, xt[s], ht[s], g_t, b_t, s)
            P.dma("sp", K.hown[i * 128:(i + 1) * 128, :], ht[s])
        P.end_phase()


def phase_gather(K):
    P = K.P

    def ccf(e, i=K.hown.ap, o=K.hfull.ap):
        return e.collective_compute("AllGather", ALU.bypass, replica_groups=[list(range(8))], ins=[i], outs=[o])
    P.cc("pool", ccf, K.hown, K.hfull)


def phase_out(K, out):
    P, TT = K.P, K.TT
    P.begin_phase()
    with ExitStack() as st:
        xt = sbt(P, st, "ox", [128, D], F32, n=2)
        for i in range(TT // 128):
            s = i % 2
            P.dma("sp", xt[s], K.hown[i * 128:(i + 1) * 128, :])
            P.dma("sp", out[i * 128:(i + 1) * 128, :], xt[s])


def rms_heads(P, K, W, x, H, dh, g_bc, out, s):
    n = H * dh
    P.call("dve", "tensor_tensor", out=W.sq[s][:, 0:n], in0=x, in1=x, op=ALU.mult)
    P.call("dve", "tensor_reduce", out=W.ss[s][:, 0:H], in_=W.sq[s][:, 0:n].re("p (h d) -> p h d", h=H), axis=AX.X, op=ALU.add)
    P.call("act", "activation", out=W.ss[s][:, 0:H], in_=W.ss[s][:, 0:H], func=AF.Sqrt, bias=K.eps_rms, scale=1.0 / dh)
    P.call("dve", "reciprocal", out=W.ss[s][:, 0:H], in_=W.ss[s][:, 0:H])
    x3 = x.re("p (h d) -> p h d", h=H)
    o3 = out.re("p (h d) -> p h d", h=H)
    P.call("dve", "tensor_tensor", out=o3, in0=x3, in1=W.ss[s][:, 0:H].bc(2, [128, H, dh]), op=ALU.mult)
    P.call("pool", "tensor_tensor", out=o3, in0=o3, in1=g_bc.bc(1, [128, H, dh]), op=ALU.mult)


def rope_heads(P, K, W, x, H, dh, cosT, sinT, out, s):
    n = H * dh
    q = dh // 4
    x3 = x.re("p (h d) -> p h d", h=H)
    t1 = W.t1[s][:, 0:n]
    t2 = W.t2[s][:, 0:n]
    P.call("dve", "tensor_tensor", out=t1.re("p (h d) -> p h d", h=H), in0=x3, in1=cosT.bc(1, [128, H, dh]), op=ALU.mult)
    x5 = x.re("p (h r f d) -> p h r f d", h=H, r=2, f=2)
    t5 = t2.re("p (h r f d) -> p h r f d", h=H, r=2, f=2)
    s4 = sinT.re("p (r f d) -> p r f d", r=2, f=2)
    for f in range(2):
        P.call("pool", "tensor_tensor", out=t5[:, :, :, f, :], in0=x5[:, :, :, 1 - f, :],
               in1=s4[:, :, f, :].bc(1, [128, H, 2, q]), op=ALU.mult)
    P.call("dve", "tensor_tensor", out=out, in0=t1, in1=t2, op=ALU.add)


def load_hfull(K, cand, dst, n0, s):
    P, S, TT = K.P, K.S, K.TT
    pieces = []
    n = n0
    while n < n0 + 128:
        r = n // TT
        m = min(n0 + 128, (r + 1) * TT)
        pieces.append((n, m))
        n = m
    for b in range(4):
        for (a, e) in pieces:
            r = a // TT
            row = (2 * b + r) * TT + (a - r * TT)
            P.dma("sp", cand[s][a - n0:e - n0, b, :], K.hfull[row:row + (e - a), :])
    P.call("dve", "tensor_scalar", out=dst, in0=cand[s][:, 0, :], scalar1=K.cmask[:, 3:4], scalar2=None, op0=ALU.mult)
    for b in range(1, 4):
        P.call("dve", "scalar_tensor_tensor", out=dst, in0=cand[s][:, b, :], scalar=K.cmask[:, 3 + b:4 + b], in1=dst, op0=ALU.mult, op1=ALU.add)


def phase_A(K):
    P, I, S, TT, l = K.P, K.I, K.S, K.TT, K.l
    P.begin_phase()
    with ExitStack() as st:
        w_in = load_w_bf(P, st, "w_in", I["w_in"][l], 8, NCOL)
        wf = load_w_bf(P, st, "wf", I["w_f"][l], 2, 256)
        bcb = load_w_bf(P, st, "bcb", I["bc"], 2, 256)
        bsb = load_w_bf(P, st, "bsb", I["bsn"], 2, 256)
        w_uq = load_w_bf(P, st, "w_uq", I["w_uq"][l], 2, 384)
        w_ukv = load_w_bf(P, st, "w_ukv", I["w_ukv"][l], 1, 512)
        W12 = sbt(P, st, "W12", [128, 2, 512], BF16)
        for m in range(2):
            for (j, mat) in enumerate((bcb, bsb)):
                pp = next_pmm(K)
                for k in range(2):
                    P.call("pe", "matmul", out=pp[:, 0:256], lhsT=mat[:, k, m * 128:(m + 1) * 128], rhs=wf[:, k, :], start=(k == 0), stop=(k == 1))
                P.call("act", "activation", out=W12[:, m, j * 256:(j + 1) * 256], in_=pp[:, 0:256], func=AF.Copy)
        qg = bcast_row(P, K, st, "qg", I["q_norm_g"][l], 64)
        kg = bcast_row(P, K, st, "kg", I["k_norm_g"][l], 64)
        mqg = bcast_row(P, K, st, "mqg", I["mla_q_norm_g"][l], 256)
        mkg = bcast_row(P, K, st, "mkg", I["mla_kv_norm_g"][l], 128)
        P.call("dve", "tensor_scalar", out=qg, in0=qg, scalar1=0.125, scalar2=None, op0=ALU.mult)
        P.call("dve", "tensor_scalar", out=mqg, in0=mqg, scalar1=96.0 ** -0.5, scalar2=None, op0=ALU.mult)

        W = KB()
        W.sq = sbt(P, st, "Wsq", [128, 256], F32, n=2)
        W.ss = sbt(P, st, "Wss", [128, 8], F32, n=2)
        W.t1 = sbt(P, st, "Wt1", [128, 256], F32, n=2)
        W.t2 = sbt(P, st, "Wt2", [128, 256], F32, n=2)
        xt = sbt(P, st, "Axt", [128, D], F32, n=2)
        xb = sbt(P, st, "Axb", [128, D], BF16, n=2)
        hTs = sbt(P, st, "AhT", [128, 8, 512], BF16, n=2)
        zq = sbt(P, st, "Azq", [128, 512], F32, n=2)
        rp = sbt(P, st, "Arp", [128, 192], F32, n=2)
        nrm = sbt(P, st, "Anrm", [128, 256], F32, n=2)
        qb = sbt(P, st, "Aqb", [128, 256], BF16, n=2)
        qTb = sbt(P, st, "AqTb", [128, 2, 512], BF16, n=2)
        cqb = sbt(P, st, "Acqb", [128, 256], BF16, n=2)
        cqT = sbt(P, st, "AcqT", [128, 2, 128], BF16, n=2)
        qm = sbt(P, st, "Aqm", [128, 384], F32, n=2)
        qmb = sbt(P, st, "Aqmb", [128, 384], BF16, n=2)
        qmTb = sbt(P, st, "AqmTb", [128, 4, 512], BF16, n=2)
        sg = sbt(P, st, "Asg", [128, 512], F32, n=2)
        ub = sbt(P, st, "Aub", [128, 2, 512], F32, n=2)

        ap_ = K.cfg.get('a_parts', ('A1', 'A2', 'A3'))
        a2p = K.cfg.get('a2_parts', ('gk', 'gv', 'mkv', 'kr', 'fz'))
        for blk in range(TT // 512 if 'A1' in ap_ else 0):
            hTb = hTs[blk % 2]
            bs = blk % 2
            for j in range(4):
                i = blk * 4 + j
                s = i % 2
                P.dma("sp", xt[s], K.hown[i * 128:(i + 1) * 128, :])
                P.dma("sp", rp[s], I["ropeown"][i * 128:(i + 1) * 128, :])
                P.call("act", "activation", out=xb[s], in_=xt[s], func=AF.Copy)
                transpose_to(P, K, xb[s], hTb[:, :, j * 128:(j + 1) * 128], 8)
                pp = next_pmm(K)
                for (o, c0) in ((0, 768), (256, 1280)):
                    for k in range(8):
                        P.call("pe", "matmul", out=pp[:, o:o + 256], lhsT=hTb[:, k, j * 128:(j + 1) * 128], rhs=w_in[:, k, c0:c0 + 256], start=(k == 0), stop=(k == 7))
                P.call("act", "activation", out=zq[s], in_=pp, func=AF.Copy)
                rms_heads(P, K, W, zq[s][:, 0:256], 4, 64, qg, nrm[s], s)
                rope_heads(P, K, W, nrm[s], 4, 64, rp[s][:, 0:64], rp[s][:, 64:128], qb[s], s)
                transpose_to(P, K, qb[s], qTb[bs][:, :, j * 128:(j + 1) * 128], 2, evac="act")
                cq = zq[s][:, 256:512]
                rms_heads(P, K, W, cq, 1, 256, mqg, nrm[s], s)
                P.call("act", "activation", out=cqb[s], in_=nrm[s], func=AF.Copy)
                transpose_to(P, K, cqb[s], cqT[s], 2)
                pp = next_pmm(K)
                for c in range(2):
                    P.call("pe", "matmul", out=pp[:, 0:384], lhsT=cqT[s][:, c, :], rhs=w_uq[:, c, :], start=(c == 0), stop=(c == 1))
                P.call("act", "activation", out=qm[s], in_=pp[:, 0:384], func=AF.Copy)
                qm3 = qm[s].re("p (h d) -> p h d", h=4)
                qmb3 = qmb[s].re("p (h d) -> p h d", h=4)
                P.call("act", "activation", out=qmb3[:, :, 0:64], in_=qm3[:, :, 0:64], func=AF.Copy)
                P.call("dve", "tensor_copy", out=nrm[s][:, 0:128].re("p (h d) -> p h d", h=4), in_=qm3[:, :, 64:96])
                rope_heads(P, K, W, nrm[s][:, 0:128], 4, 32, rp[s][:, 128:160], rp[s][:, 160:192], nrm[s][:, 128:256], s)
                P.call("dve", "tensor_copy", out=qmb3[:, :, 64:96], in_=nrm[s][:, 128:256].re("p (h d) -> p h d", h=4))
                pt = next_ptr(K)
                for h in range(4):
                    P.call("pe", "transpose", out=pt[0:96, h, :], in_=qmb[s][:, h * 96:(h + 1) * 96], identity=K.idb)
                P.call("dve", "tensor_copy", out=qmTb[bs][0:96, :, j * 128:(j + 1) * 128], in_=pt[0:96, 0:4, :])
            t0, t1_ = blk * 512, (blk + 1) * 512
            P.dma("sp", K.QTg.re("(c p) t -> p c t", p=128)[:, :, t0:t1_], qTb[bs])
            for h in range(4):
                P.dma("sp", K.QTm[h * 96:(h + 1) * 96, t0:t1_], qmTb[bs][0:96, h, :])
            pps = [next_pmm(K) for _ in range(4)]
            for c4 in range(4):
                for k in range(8):
                    P.call("pe", "matmul", out=pps[c4], lhsT=w_in[:, k, 256 + c4 * 128:256 + (c4 + 1) * 128], rhs=hTb[:, k, :], start=(k == 0), stop=(k == 7))
            for c in range(2):
                P.call("act", "activation", out=sg[c], in_=pps[2 + c], func=AF.Sigmoid)
                P.call("dve", "tensor_tensor", out=ub[bs][:, c, :], in0=pps[c], in1=sg[c], op=ALU.mult)
            P.dma("sp", K.uT.re("(c p) t -> p c t", p=128)[:, :, 15 + t0:15 + t1_], ub[bs])

        if 'A3' not in ap_:
            P.end_phase()
            return
        cand = sbt(P, st, "Acand", [128, 4, D], F32, n=2)
        load_hfull(K, cand, xt[0], TT - 64, 0)
        P.call("act", "activation", out=xb[0], in_=xt[0], func=AF.Copy)
        hT1 = hTs[0][:, :, 0:128]
        transpose_to(P, K, xb[0], hT1, 8)
        pps = [next_pmm(K) for _ in range(4)]
        for c4 in range(4):
            for k in range(8):
                P.call("pe", "matmul", out=pps[c4][:, 0:128], lhsT=w_in[:, k, 256 + c4 * 128:256 + (c4 + 1) * 128], rhs=hT1[:, k, :], start=(k == 0), stop=(k == 7))
        for c in range(2):
            P.call("act", "activation", out=sg[c][:, 0:128], in_=pps[2 + c][:, 0:128], func=AF.Sigmoid)
            P.call("dve", "tensor_tensor", out=ub[0][:, c, 0:128], in0=pps[c][:, 0:128], in1=sg[c][:, 0:128], op=ALU.mult)
            P.call("dve", "tensor_scalar", out=ub[0][:, c, 128:143], in0=ub[0][:, c, 49:64], scalar1=K.cmask[:, 0:1], scalar2=None, op0=ALU.mult)
            P.call("dve", "tensor_scalar", out=ub[0][:, c, 143:158], in0=ub[0][:, c, 64:79], scalar1=K.cmask[:, 1:2], scalar2=None, op0=ALU.mult)
        uT3 = K.uT.re("(c p) t -> p c t", p=128)
        P.dma("sp", uT3[:, :, 0:15], ub[0][:, :, 128:143])
        P.dma("sp", uT3[:, :, 15 + TT:30 + TT], ub[0][:, :, 143:158])

        zk = zq
        kb = sbt(P, st, "Akb", [128, 128], BF16, n=2)
        kTb = sbt(P, st, "AkTb", [128, 512], BF16, n=2)
        va = sbt(P, st, "Ava", [128, 2, 65], BF16, n=2)
        vma = sbt(P, st, "Avma", [128, 4, 65], BF16, n=2)
        ckb = sbt(P, st, "Ackb", [128, 128], BF16, n=2)
        ckT = sbt(P, st, "AckT", [128, 1, 128], BF16, n=2)
        knb = sbt(P, st, "Aknb", [128, 256], BF16, n=2)
        knTb = sbt(P, st, "AknTb", [128, 2, 512], BF16, n=2)
        krb = sbt(P, st, "Akrb", [128, 32], BF16, n=2)
        krTb = sbt(P, st, "AkrTb", [32, 512], BF16, n=2)
        zfT = sbt(P, st, "AzfT", [128, 2, 512], BF16, n=2)
        zb = sbt(P, st, "Azb", [128, 512], BF16, n=2)
        for s in range(2):
            P.call("dve", "memset", w=[va[s]], ap=va[s].ap, constant=1.0)
            P.call("dve", "memset", w=[vma[s]], ap=vma[s].ap, constant=1.0)
        for blk in range(S // 512 if 'A2' in ap_ else 0):
            hTb = hTs[blk % 2]
            bs = blk % 2
            t0, t1_ = blk * 512, (blk + 1) * 512
            for j in range(4):
                i = blk * 4 + j
                s = i % 2
                load_hfull(K, cand, xt[s], i * 128, s)
                P.dma("sp", rp[s], I["ropefull"][i * 128:(i + 1) * 128, :])
                P.call("act", "activation", out=xb[s], in_=xt[s], func=AF.Copy)
                transpose_to(P, K, xb[s], hTb[:, :, j * 128:(j + 1) * 128], 8)
                pp = next_pmm(K)
                for (o, c0, n) in ((0, 1024, 256), (256, 1536, 160)):
                    for k in range(8):
                        P.call("pe", "matmul", out=pp[:, o:o + n], lhsT=hTb[:, k, j * 128:(j + 1) * 128], rhs=w_in[:, k, c0:c0 + n], start=(k == 0), stop=(k == 7))
                P.call("act", "activation", out=zk[s][:, 0:416], in_=pp[:, 0:416], func=AF.Copy)
                if 'gk' in a2p:
                    rms_heads(P, K, W, zk[s][:, 0:128], 2, 64, kg, nrm[s][:, 0:128], s)
                    rope_heads(P, K, W, nrm[s][:, 0:128], 2, 64, rp[s][:, 0:64], rp[s][:, 64:128], kb[s], s)
                    transpose_to(P, K, kb[s], kTb[bs][:, j * 128:(j + 1) * 128].re("p (c t) -> p c t", c=1), 1, evac="act")
                if 'gv' in a2p:
                    P.call("act", "activation", out=va[s][:, :, 0:64], in_=zk[s][:, 128:256].re("p (h d) -> p h d", h=2), func=AF.Copy)
                    P.dma("sp", K.Vg[i * 128:(i + 1) * 128, :], va[s].re("p h d -> p (h d)"))
                if 'mkv' in a2p:
                    rms_heads(P, K, W, zk[s][:, 256:384], 1, 128, mkg, nrm[s][:, 128:256], s)
                    P.call("act", "activation", out=ckb[s], in_=nrm[s][:, 128:256], func=AF.Copy)
                    transpose_to(P, K, ckb[s], ckT[s], 1)
                    pp = next_pmm(K)
                    P.call("pe", "matmul", out=pp, lhsT=ckT[s][:, 0, :], rhs=w_ukv[:, 0, :], start=True, stop=True)
                    kv3 = pp.re("p (h d) -> p h d", h=4)
                    if 'nov' not in a2p:
                        P.call("act", "activation", out=vma[s][:, :, 0:64], in_=kv3[:, :, 64:128], func=AF.Copy)
                        P.dma("sp", K.Vm[i * 128:(i + 1) * 128, :], vma[s].re("p h d -> p (h d)"))
                    if 'nok' not in a2p:
                        P.call("dve", "tensor_copy", out=knb[s].re("p (h d) -> p h d", h=4), in_=kv3[:, :, 0:64])
                        transpose_to(P, K, knb[s], knTb[bs][:, :, j * 128:(j + 1) * 128], 2)
                if 'kr' in a2p:
                    rope_heads(P, K, W, zk[s][:, 384:416], 1, 32, rp[s][:, 128:160], rp[s][:, 160:192], krb[s], s)
                    pt = next_ptr(K)
                    P.call("pe", "transpose", out=pt[0:32, 0, :], in_=krb[s], identity=K.idb)
                    P.call("act", "activation", out=krTb[bs][:, j * 128:(j + 1) * 128], in_=pt[0:32, 0, :], func=AF.Copy)
            if 'gk' in a2p:
                P.dma("sp", K.KTg[:, t0:t1_], kTb[bs])
            if 'mkv' in a2p:
                P.dma("sp", K.KTm.re("(c p) t -> p c t", p=128)[:, :, t0:t1_], knTb[bs])
            if 'kr' in a2p:
                P.dma("sp", K.KRm[:, t0:t1_], krTb[bs])
            if 'fz' in a2p:
                for c in range(2):
                    pp = next_pmm(K)
                    for k in range(8):
                        P.call("pe", "matmul", out=pp, lhsT=w_in[:, k, c * 128:(c + 1) * 128], rhs=hTb[:, k, :], start=(k == 0), stop=(k == 7))
                    P.call("act", "activation", out=zfT[bs][:, c, :], in_=pp, func=AF.Copy)
                for j in range(4):
                    i = blk * 4 + j
                    s = i % 2
                    pp = next_pmm(K)
                    for c in range(2):
                        P.call("pe", "matmul", out=pp, lhsT=zfT[bs][:, c, j * 128:(j + 1) * 128], rhs=W12[:, c, :], start=(c == 0), stop=(c == 1))
                    P.call("dve", "tensor_copy", out=zb[s], in_=pp)
                    P.dma("sp", K.Zd[i * 128:(i + 1) * 128, :], zb[s])
        P.end_phase()


def phase_fnet(K):
    P, I, S, TT, l = K.P, K.I, K.S, K.TT, K.l
    NCH = S // 128
    P.begin_phase()
    with ExitStack() as st:
        Zsb = sbt(P, st, "Zsb", [128, NCH, 512], BF16)
        Zv = K.Zd.re("(n p) c -> p n c", p=128)
        for c0 in range(0, NCH, 8):
            P.dma("sp", Zsb[:, c0:c0 + 8, :], Zv[:, c0:c0 + 8, :])
        bf_t = bcast_row(P, K, st, "bf", I["b_f"][l], 256)
        cs = sbt(P, st, "Fcs", [128, 8, 512], BF16, n=2)
        sn = sbt(P, st, "Fsn", [128, 8, 512], BF16, n=2)
        yt = sbt(P, st, "Fy", [128, 256], F32, n=2)
        Cv = K.dft[0].re("(n p) k -> p n k", p=128)
        Sv = K.dft[1].re("(n p) k -> p n k", p=128)
        for c0 in range(0, NCH, 8):
            P.call("dve", "tensor_scalar", out=Zsb[:, c0:c0 + 8, :], in0=Zsb[:, c0:c0 + 8, :], scalar1=K.cmask[:, 2:3], scalar2=None, op0=ALU.mult)
        NP = NCH // 8
        it = 0
        for kb in range(TT // 512):
            pps = [K.pacc[0], K.pacc[1], K.pmm[0], K.pmm[1]]
            for pc in range(NP):
                s = it % 2
                it += 1
                P.dma("sp", cs[s], Cv[:, pc * 8:(pc + 1) * 8, kb * 512:(kb + 1) * 512])
                P.dma("sp", sn[s], Sv[:, pc * 8:(pc + 1) * 8, kb * 512:(kb + 1) * 512])
                for nn in range(8):
                    n = pc * 8 + nn
                    for j in range(4):
                        pp = pps[j]
                        o = 0
                        P.call("pe", "matmul", out=pp[:, o:o + 256], lhsT=cs[s][:, nn, j * 128:(j + 1) * 128], rhs=Zsb[:, n, 0:256], start=(n == 0), stop=False)
                        P.call("pe", "matmul", out=pp[:, o:o + 256], lhsT=sn[s][:, nn, j * 128:(j + 1) * 128], rhs=Zsb[:, n, 256:512], start=False, stop=(n == NCH - 1))
            for j in range(4):
                s2 = j % 2
                o = 0
                P.call("dve", "tensor_tensor", out=yt[s2], in0=pps[j][:, o:o + 256], in1=bf_t, op=ALU.add)
                i = kb * 4 + j
                P.dma("sp", K.Y[i * 128:(i + 1) * 128, 0:256], yt[s2])
        P.end_phase()


def phase_conv(K):
    P, I, S, TT, l = K.P, K.I, K.S, K.TT, K.l
    P.begin_phase()
    with ExitStack() as st:
        u = sbt(P, st, "Cu", [128, 2, TT + 30], F32)
        P.dma("sp", u, K.uT.re("(c p) t -> p c t", p=128)[:, :, 0:TT + 30])
        dw = sbt(P, st, "Cdw", [128, 2, 31], F32)
        for c in range(2):
            P.dma("sp", dw[:, c, :], I["dw_w"][l][:, c * 128:(c + 1) * 128].re("j p -> p j"), allow_slow_non_contiguous=True)
        sm = sbt(P, st, "Csm", [128, 3, 2], F32)
        P.dma("sp", sm[:, 0, :], I["dw_b"][l].re("(c p) -> p c", p=128), allow_slow_non_contiguous=True)
        P.dma("sp", sm[:, 1, :], I["conv_ln_g"][l].re("(c p) -> p c", p=128), allow_slow_non_contiguous=True)
        P.dma("sp", sm[:, 2, :], I["conv_ln_b"][l].re("(c p) -> p c", p=128), allow_slow_non_contiguous=True)
        wpw = load_w_bf(P, st, "wpw", I["w_pw"][l], 2, 256)
        bpw = bcast_row(P, K, st, "bpw", I["b_pw"][l], 256)
        CW = min(1024, TT)
        NCW = TT // CW
        accs = [[sbt(P, st, f"Cacc{c}_{w}", [128, CW], F32) for w in range(NCW)] for c in range(2)]
        for w in range(NCW):
            for c in range(2):
                acc = accs[c][w]
                o = w * CW
                P.call("dve", "tensor_scalar", out=acc, in0=u[:, c, o:o + CW], scalar1=dw[:, c, 0:1], scalar2=sm[:, 0, c:c + 1], op0=ALU.mult, op1=ALU.add)
                for j in range(1, 31):
                    P.call("dve", "scalar_tensor_tensor", out=acc, in0=u[:, c, o + j:o + j + CW], scalar=dw[:, c, j:j + 1], in1=acc, op0=ALU.mult, op1=ALU.add)
        sq = sbt(P, st, "Csq", [128, 2, 512], F32, n=2)
        mt = sbt(P, st, "Cmt", [128, 512], F32, n=2)
        vt = sbt(P, st, "Cvt", [128, 512], F32, n=2)
        xn = sbt(P, st, "Cxn", [128, 2, 512], F32, n=2)
        sT = sbt(P, st, "CsT", [128, 2, 512], BF16, n=2)
        yt = sbt(P, st, "Cy", [128, 256], F32, n=2)
        for tb in range(TT // 512):
            s = tb % 2
            w_ = (tb * 512) // CW
            sl = slice(tb * 512 - w_ * CW, tb * 512 - w_ * CW + 512)
            accw = [accs[0][w_], accs[1][w_]]
            pm = next_pmm(K)
            pq = next_pmm(K)
            for c in range(2):
                P.call("act", "activation", out=sq[s][:, c, :], in_=accw[c][:, sl], func=AF.Square)
            for c in range(2):
                P.call("pe", "matmul", out=pm, lhsT=K.ones_f, rhs=accw[c][:, sl], start=(c == 0), stop=(c == 1))
            for c in range(2):
                P.call("pe", "matmul", out=pq, lhsT=K.ones_f, rhs=sq[s][:, c, :], start=(c == 0), stop=(c == 1))
            P.call("act", "activation", out=mt[s], in_=pm, func=AF.Copy, scale=1.0 / 256)
            P.call("dve", "tensor_tensor", out=vt[s], in0=mt[s], in1=mt[s], op=ALU.mult)
            P.call("dve", "scalar_tensor_tensor", out=vt[s], in0=pq, scalar=1.0 / 256, in1=vt[s], op0=ALU.mult, op1=ALU.subtract)
            P.call("act", "activation", out=vt[s], in_=vt[s], func=AF.Sqrt, bias=K.eps_ln, scale=1.0)
            P.call("dve", "reciprocal", out=vt[s], in_=vt[s])
            for c in range(2):
                P.call("dve", "tensor_tensor", out=xn[s][:, c, :], in0=accw[c][:, sl], in1=mt[s], op=ALU.subtract)
                P.call("pool", "tensor_tensor", out=xn[s][:, c, :], in0=xn[s][:, c, :], in1=vt[s], op=ALU.mult)
                P.call("dve", "tensor_scalar", out=xn[s][:, c, :], in0=xn[s][:, c, :], scalar1=sm[:, 1, c:c + 1], scalar2=sm[:, 2, c:c + 1], op0=ALU.mult, op1=ALU.add)
                P.call("act", "activation", out=sT[s][:, c, :], in_=xn[s][:, c, :], func=AF.Silu)
            for j in range(4):
                s2 = j % 2
                pp = next_pmm(K)
                for c in range(2):
                    P.call("pe", "matmul", out=pp[:, 0:256], lhsT=sT[s][:, c, j * 128:(j + 1) * 128], rhs=wpw[:, c, :], start=(c == 0), stop=(c == 1))
                P.call("dve", "tensor_tensor", out=yt[s2], in0=pp[:, 0:256], in1=bpw, op=ALU.add)
                i = tb * 4 + j
                P.dma("sp", K.Y[i * 128:(i + 1) * 128, 256:512], yt[s2])
        P.end_phase()


def attn_block(K, kt, va, qt, dk, NK, pt, ot, yo, rc, ydst_fn):
    P = K.P
    po = K.pacc[K.ai % 2]
    K.ai += 1

    def qk(kc):
        ps = next_pmm(K)
        P.call("pe", "matmul", out=ps, lhsT=kt[0:dk, kc * 128:(kc + 1) * 128], rhs=qt[0:dk, :], start=True, stop=True)
        return ps
    ps_next = qk(0)
    for kc in range(NK):
        ps = ps_next
        if kc + 1 < NK:
            ps_next = qk(kc + 1)
        p_ = pt[kc % 3]
        P.call("act", "activation", out=p_, in_=ps, func=AF.Exp)
        P.call("pe", "matmul", out=po[0:65, :], lhsT=va[:, kc, :], rhs=p_, start=(kc == 0), stop=(kc == NK - 1))
    P.call("act", "activation", out=ot[0:65, :], in_=po[0:65, :], func=AF.Copy)
    for j in range(4):
        s2 = j % 2
        pf = next_pmm(K)
        P.call("pe", "transpose", out=pf[:, 0:65], in_=ot[0:65, j * 128:(j + 1) * 128], identity=K.idf[0:65, 0:65])
        P.call("dve", "reciprocal", out=rc[s2], in_=pf[:, 64:65])
        P.call("dve", "tensor_scalar", out=yo[s2], in0=pf[:, 0:64], scalar1=rc[s2], scalar2=None, op0=ALU.mult)
        P.dma("sp", ydst_fn(j), yo[s2])


def phase_attn(K):
    P, I, S, TT, l = K.P, K.I, K.S, K.TT, K.l
    NK = S // 128
    K.ai = 0
    P.begin_phase()
    with ExitStack() as st:
        kt = sbt(P, st, "Tkt", [96, S], BF16, n=2)
        va = sbt(P, st, "Tva", [128, NK, 65], BF16, n=2)
        qt = sbt(P, st, "Tqt", [96, 512], BF16, n=2)
        pt = sbt(P, st, "Tpt", [128, 512], BF16, n=3)
        ot = sbt(P, st, "Tot", [65, 512], F32, n=2)
        yo = sbt(P, st, "Tyo", [128, 64], F32, n=2)
        rc = sbt(P, st, "Trc", [128, 1], F32, n=2)
        heads = [("g", h) for h in range(4)] + [("m", h) for h in range(4)]
        qi = 0
        for hi, (kind, h) in enumerate(heads):
            s = hi % 2
            if kind == "g":
                dk = 64
                kvh = h // 2
                P.dma("sp", kt[s][0:64, :], K.KTg[kvh * 64:(kvh + 1) * 64, :])
                vsrc = K.Vg.re("(n p) (h d) -> p n h d", p=128, h=2)
                for c0 in range(0, NK, 8):
                    P.dma("sp", va[s][:, c0:c0 + 8, :], vsrc[:, c0:c0 + 8, kvh, :])
                qsrc = K.QTg[h * 64:(h + 1) * 64, :]
                ycol = 512 + h * 64
            else:
                dk = 96
                P.dma("sp", kt[s][0:64, :], K.KTm[h * 64:(h + 1) * 64, :])
                P.dma("sp", kt[s][64:96, :], K.KRm[:, :])
                vsrc = K.Vm.re("(n p) (h d) -> p n h d", p=128, h=4)
                for c0 in range(0, NK, 8):
                    P.dma("sp", va[s][:, c0:c0 + 8, :], vsrc[:, c0:c0 + 8, h, :])
                qsrc = K.QTm[h * 96:(h + 1) * 96, :]
                ycol = 768 + h * 64
            for qb in range(TT // 512):
                qs = qi % 2
                qi += 1
                P.dma("sp", qt[qs][0:dk, :], qsrc[:, qb * 512:(qb + 1) * 512])

                def ydst(j, qb=qb, ycol=ycol):
                    i = qb * 4 + j
                    return K.Y[i * 128:(i + 1) * 128, ycol:ycol + 64]
                attn_block(K, kt[s], va[s], qt[qs], dk, NK, pt, ot[qs], yo, rc, ydst)
        P.end_phase()


def res_ln_store(K, A, pps, hx, rt, ht, g_t, b_t, i, s):
    P = K.P
    for hlf in range(2):
        P.call("dve", "scalar_tensor_tensor", out=rt[:, hlf * 512:(hlf + 1) * 512], in0=hx[:, hlf * 512:(hlf + 1) * 512], scalar=ALPHA, in1=pps[hlf], op0=ALU.mult, op1=ALU.add)
    ln_tile(P, K, A, rt, ht, g_t, b_t, s)
    P.dma("sp", K.hnext[i * 128:(i + 1) * 128, :], ht)


def swap_h(K):
    K.hown, K.hnext = K.hnext, K.hown


def phase_wo(K):
    P, I, S, TT, l = K.P, K.I, K.S, K.TT, K.l
    P.begin_phase()
    with ExitStack() as st:
        wo = load_w_bf(P, st, "wo", I["w_o"][l], 8, 1024)
        gg = bcast_row(P, K, st, "gg", I["grp_norm_g"][l], 1024)
        g1 = bcast_row(P, K, st, "g1", I["ln1_g"][l], D)
        b1 = bcast_row(P, K, st, "b1", I["ln1_b"][l], D)
        A = ln_scratch(P, st, "wo")
        yt = sbt(P, st, "Oy", [128, D], F32, n=2)
        hx = sbt(P, st, "Oh", [128, D], F32, n=2)
        sq = sbt(P, st, "Osq", [128, D], F32, n=2)
        ss = sbt(P, st, "Oss", [128, 4], F32, n=2)
        yb = sbt(P, st, "Oyb", [128, D], BF16, n=2)
        yT = sbt(P, st, "OyT", [128, 8, 128], BF16, n=2)
        rt = sbt(P, st, "Ort", [128, D], F32, n=2)
        ht = sbt(P, st, "Oht", [128, D], F32, n=2)
        for i in range(TT // 128):
            s = i % 2
            P.dma("sp", yt[s], K.Y[i * 128:(i + 1) * 128, :])
            P.dma("sp", hx[s], K.hown[i * 128:(i + 1) * 128, :])
            P.call("dve", "tensor_tensor", out=sq[s], in0=yt[s], in1=yt[s], op=ALU.mult)
            P.call("dve", "tensor_reduce", out=ss[s], in_=sq[s].re("p (h d) -> p h d", h=4), axis=AX.X, op=ALU.add)
            P.call("act", "activation", out=ss[s], in_=ss[s], func=AF.Sqrt, bias=K.eps_rms, scale=1.0 / 256)
            P.call("dve", "reciprocal", out=ss[s], in_=ss[s])
            P.call("dve", "tensor_tensor", out=sq[s].re("p (h d) -> p h d", h=4), in0=yt[s].re("p (h d) -> p h d", h=4), in1=ss[s].bc(2, [128, 4, 256]), op=ALU.mult)
            P.call("pool", "tensor_tensor", out=yb[s], in0=sq[s], in1=gg, op=ALU.mult)
            transpose_to(P, K, yb[s], yT[s], 8)
            pps = [next_pmm(K), next_pmm(K)]
            for hlf in range(2):
                for k in range(8):
                    P.call("pe", "matmul", out=pps[hlf], lhsT=yT[s][:, k, :], rhs=wo[:, k, hlf * 512:(hlf + 1) * 512], start=(k == 0), stop=(k == 7))
            res_ln_store(K, A, pps, hx[s], rt[s], ht[s], g1, b1, i, s)
        P.end_phase()
    swap_h(K)


def phase_xattn(K):
    P, I, S, TT, l = K.P, K.I, K.S, K.TT, K.l
    P.begin_phase()
    with ExitStack() as st:
        wq = load_w_bf(P, st, "wxq", I["w_xq"][l], 8, 1024)
        wk = load_w_bf(P, st, "wxk", I["w_xk"][l], 8, 1024)
        wv = load_w_bf(P, st, "wxv", I["w_xv"][l], 8, 1024)
        wo = load_w_bf(P, st, "wxo", I["w_xo"][l], 8, 1024)
        g2 = bcast_row(P, K, st, "g2", I["ln2_g"][l], D)
        b2 = bcast_row(P, K, st, "b2", I["ln2_b"][l], D)
        A = ln_scratch(P, st, "xa")
        xt4 = sbt(P, st, "Xx4", [128, 4, D], F32, n=2)
        xb = sbt(P, st, "Xxb", [128, D], BF16, n=2)
        memT = sbt(P, st, "XmT", [128, 8, 256], BF16)
        kxT = sbt(P, st, "XkT", [128, 8, 256], BF16)
        vx = sbt(P, st, "Xvx", [128, 2, 1024], BF16)
        for i in range(2):
            P.dma("sp", xt4[i][:, 0, :], I["mem"][i * 128:(i + 1) * 128, :])
            P.call("act", "activation", out=xb[i], in_=xt4[i][:, 0, :], func=AF.Copy)
            transpose_to(P, K, xb[i], memT[:, :, i * 128:(i + 1) * 128], 8)
        for c in range(8):
            pp = next_pmm(K)
            for k in range(8):
                P.call("pe", "matmul", out=pp[:, 0:256], lhsT=wk[:, k, c * 128:(c + 1) * 128], rhs=memT[:, k, :], start=(k == 0), stop=(k == 7))
            P.call("act", "activation", out=kxT[:, c, :], in_=pp[:, 0:256], func=AF.Copy)
        for kt_ in range(2):
            for hlf in range(2):
                pp = next_pmm(K)
                for k in range(8):
                    P.call("pe", "matmul", out=pp, lhsT=memT[:, k, kt_ * 128:(kt_ + 1) * 128], rhs=wv[:, k, hlf * 512:(hlf + 1) * 512], start=(k == 0), stop=(k == 7))
                P.call("dve", "tensor_copy", out=vx[:, kt_, hlf * 512:(hlf + 1) * 512], in_=pp)
        hTs = sbt(P, st, "XhT", [128, 8, 512], BF16, n=2)
        qxT = sbt(P, st, "XqT", [128, 8, 512], BF16)
        pT = sbt(P, st, "XpT", [128, 2, 512], BF16, n=2)
        rden = sbt(P, st, "Xrd", [128, 512], F32, n=2)
        xaT = sbt(P, st, "XaT", [128, 8, 512], BF16, n=2)
        rt = sbt(P, st, "Xrt", [128, D], F32, n=2)
        ht = sbt(P, st, "Xht", [128, D], F32, n=2)
        for blk in range(TT // 512):
            bs = blk % 2
            hTb = hTs[bs]
            for j in range(4):
                i = blk * 4 + j
                s = i % 2
                P.dma("sp", xt4[bs][:, j, :], K.hown[i * 128:(i + 1) * 128, :])
                P.call("act", "activation", out=xb[s], in_=xt4[bs][:, j, :], func=AF.Copy)
                transpose_to(P, K, xb[s], hTb[:, :, j * 128:(j + 1) * 128], 8)
            for c in range(8):
                pp = next_pmm(K)
                for k in range(8):
                    P.call("pe", "matmul", out=pp, lhsT=wq[:, k, c * 128:(c + 1) * 128], rhs=hTb[:, k, :], start=(k == 0), stop=(k == 7))
                P.call("act", "activation", out=qxT[:, c, :], in_=pp, func=AF.Copy, scale=1.0 / 16)
            for h in range(4):
                hs = h % 2
                for kt_ in range(2):
                    ps = next_pmm(K)
                    for dc in range(2):
                        P.call("pe", "matmul", out=ps, lhsT=kxT[:, 2 * h + dc, kt_ * 128:(kt_ + 1) * 128], rhs=qxT[:, 2 * h + dc, :], start=(dc == 0), stop=(dc == 1))
                    P.call("act", "activation", out=pT[hs][:, kt_, :], in_=ps, func=AF.Exp)
                pd = next_pmm(K)
                for kt_ in range(2):
                    P.call("pe", "matmul", out=pd, lhsT=K.ones_b, rhs=pT[hs][:, kt_, :], start=(kt_ == 0), stop=(kt_ == 1))
                P.call("dve", "reciprocal", out=rden[hs], in_=pd)
                for dc in range(2):
                    po = next_pmm(K)
                    for kt_ in range(2):
                        P.call("pe", "matmul", out=po, lhsT=vx[:, kt_, h * 256 + dc * 128:h * 256 + (dc + 1) * 128], rhs=pT[hs][:, kt_, :], start=(kt_ == 0), stop=(kt_ == 1))
                    P.call("dve", "tensor_tensor", out=xaT[bs][:, 2 * h + dc, :], in0=po, in1=rden[hs], op=ALU.mult)
            for j in range(4):
                i = blk * 4 + j
                s = i % 2
                pps = [K.pacc[0], K.pacc[1]]
                for hlf in range(2):
                    for c in range(8):
                        P.call("pe", "matmul", out=pps[hlf], lhsT=xaT[bs][:, c, j * 128:(j + 1) * 128], rhs=wo[:, c, hlf * 512:(hlf + 1) * 512], start=(c == 0), stop=(c == 7))
                res_ln_store(K, A, pps, xt4[bs][:, j, :], rt[s], ht[s], g2, b2, i, s)
        P.end_phase()
    swap_h(K)


def phase_moe(K):
    P, I, S, TT, l = K.P, K.I, K.S, K.TT, K.l
    NT = TT // 128
    wgub = K.wgub
    wdnb = K.wdnb
    P.begin_phase()
    with ExitStack() as st:
        sf = sbt(P, st, "Mcf", [128, 2048], F32, n=3)
        sb_ = sbt(P, st, "Mcb", [128, 2048], BF16, n=3)
        it = 0
        for (src, dst, ncol) in ((K.wgu[l], wgub, 2048), (K.wdn[l], wdnb, 1024)):
            for r in range(NE * D // 128):
                s = it % 3
                P.dma("sp", sf[s][:, 0:ncol], src[r * 128:(r + 1) * 128, :])
                eng = ("act", "dve", "pool")[it % 3]
                if eng == "act":
                    P.call("act", "activation", out=sb_[s][:, 0:ncol], in_=sf[s][:, 0:ncol], func=AF.Copy)
                else:
                    P.call(eng, "tensor_copy", out=sb_[s][:, 0:ncol], in_=sf[s][:, 0:ncol])
                P.dma("sp", dst[r * 128:(r + 1) * 128, :], sb_[s][:, 0:ncol])
                it += 1
        P.end_phase()

    P.begin_phase()
    with ExitStack() as st:
        g3 = bcast_row(P, K, st, "g3", I["ln3_g"][l], D)
        b3 = bcast_row(P, K, st, "b3", I["ln3_b"][l], D)
        A = ln_scratch(P, st, "mo")
        G = sbt(P, st, "MG", [128, NT, NE], F32)
        GT = sbt(P, st, "MGT", [NE, NT, 128], BF16)
        bdn = sbt(P, st, "Mbdn", [NE, D], BF16)
        bgu = sbt(P, st, "Mbgu", [128, 16, NE], F32)
        xt = sbt(P, st, "Mxt", [128, D], F32, n=2)
        xb = sbt(P, st, "Mxb", [128, D], BF16, n=2)
        st1 = ExitStack()
        brt = bcast_row(P, K, st1, "brt", I["b_router"][l], NE)
        wr = sbt(P, st1, "Mwr", [128, 8, NE], F32)
        P.dma("sp", wr, I["w_router"][l].re("(k p) e -> p k e", p=128))
        bdnf = sbt(P, st1, "Mbdnf", [NE, D], F32)
        P.dma("sp", bdnf, I["b_down"][l])
        P.call("dve", "tensor_copy", out=bdn, in_=bdnf)
        bgt = sbt(P, st1, "Mbgt", [NE, 2 * D], F32)
        P.dma("sp", bgt, I["b_gu"][l])
        for c in range(16):
            pf = next_pmm(K)
            P.call("pe", "transpose", out=pf[:, 0:NE], in_=bgt[0:NE, c * 128:(c + 1) * 128], identity=K.idf[0:NE, 0:NE])
            P.call("dve", "tensor_copy", out=bgu[:, c, :], in_=pf[:, 0:NE])
        hTf = sbt(P, st1, "MhTf", [128, 8, 128], F32)
        lg = sbt(P, st1, "Mlg", [128, NE], F32, n=2)
        mx = sbt(P, st1, "Mmx", [128, 8], F32, n=2)
        ex = sbt(P, st1, "Mex", [128, NE], F32, n=2)
        sm = sbt(P, st1, "Msm", [128, 2], F32, n=2)
        for i in range(NT):
            s = i % 2
            P.dma("sp", xt[s], K.hown[i * 128:(i + 1) * 128, :])
            for half in range(2):
                pf = next_pmm(K)
                for kk in range(4):
                    k = half * 4 + kk
                    P.call("pe", "transpose", out=pf[:, kk * 128:(kk + 1) * 128], in_=xt[s][:, k * 128:(k + 1) * 128], identity=K.idf)
                P.call("dve" if half == 0 else "act", "tensor_copy" if half == 0 else "activation",
                       **(dict(out=hTf[:, half * 4:half * 4 + 4, :], in_=pf.re("p (k t) -> p k t", k=4)) if half == 0 else
                          dict(out=hTf[:, half * 4:half * 4 + 4, :], in_=pf.re("p (k t) -> p k t", k=4), func=AF.Copy)))
            pl = next_pmm(K)
            for k in range(8):
                P.call("pe", "matmul", out=pl[:, 0:NE], lhsT=hTf[:, k, :], rhs=wr[:, k, :], start=(k == 0), stop=(k == 7))
            P.call("dve", "tensor_tensor", out=lg[s], in0=pl[:, 0:NE], in1=brt, op=ALU.add)
            P.call("dve", "max", out=mx[s], in_=lg[s])
            P.call("dve", "tensor_scalar", out=sm[s][:, 0:1], in0=mx[s][:, 0:1], scalar1=-1.0, scalar2=None, op0=ALU.mult)
            P.call("act", "activation", out=ex[s], in_=lg[s], func=AF.Exp, bias=sm[s][:, 0:1], scale=1.0)
            P.call("dve", "tensor_scalar", out=lg[s], in0=lg[s], scalar1=mx[s][:, 3:4], scalar2=None, op0=ALU.is_ge)
            P.call("dve", "tensor_tensor", out=ex[s], in0=ex[s], in1=lg[s], op=ALU.mult)
            P.call("dve", "tensor_reduce", out=sm[s][:, 1:2], in_=ex[s].re("p (o e) -> p o e", o=1), axis=AX.X, op=ALU.add)
            P.call("dve", "reciprocal", out=sm[s][:, 1:2], in_=sm[s][:, 1:2])
            P.call("dve", "tensor_scalar", out=G[:, i, :], in0=ex[s], scalar1=sm[s][:, 1:2], scalar2=None, op0=ALU.mult)
            pf = next_pmm(K)
            P.call("pe", "transpose", out=pf[0:NE, 0:128], in_=G[:, i, :], identity=K.idf)
            P.call("act", "activation", out=GT[:, i, :], in_=pf[0:NE, 0:128], func=AF.Copy)

        P.barrier()
        st1.close()

        PT = min(512, TT)
        NTP = PT // 128
        NBP = PT // 512
        hT = sbt(P, st, "MhT", [128, 8, PT], BF16)
        acc = sbt(P, st, "Macc", [128, NTP, D], F32)
        wg = sbt(P, st, "Mwg", [128, 8, 2 * D], BF16, n=2)
        wd = sbt(P, st, "Mwd", [128, 8, D], BF16, n=2)
        actT = sbt(P, st, "MaT", [128, 8, 512], BF16, n=2)
        gt = sbt(P, st, "Mgt", [128, 512], F32, n=2)
        sg = sbt(P, st, "Msg", [128, 512], F32, n=2)
        ut = sbt(P, st, "Mut", [128, 512], F32, n=2)
        rt = sbt(P, st, "Mrt", [128, D], F32)
        ht = sbt(P, st, "Mht", [128, D], F32)
        wgv = wgub.re("(e k p) c -> e p k c", p=128, k=8)
        wdv = wdnb.re("(e k p) c -> e p k c", p=128, k=8)
        wi = 0
        for ps_ in range(TT // PT):
            t0 = ps_ * NTP
            for j in range(NTP):
                i = t0 + j
                s = i % 2
                P.dma("sp", xt[s], K.hown[i * 128:(i + 1) * 128, :])
                P.call("act", "activation", out=xb[s], in_=xt[s], func=AF.Copy)
                transpose_to(P, K, xb[s], hT[:, :, j * 128:(j + 1) * 128], 8)
                for hlf in range(2):
                    pp = next_pmm(K)
                    P.call("pe", "matmul", out=pp, lhsT=GT[:, i, :], rhs=bdn[:, hlf * 512:(hlf + 1) * 512], start=True, stop=True)
                    P.call("act", "activation", out=acc[:, j, hlf * 512:(hlf + 1) * 512], in_=pp, func=AF.Copy)
            for e in range(NE):
                ws = wi % 2
                wi += 1
                for k0 in range(0, 8, 2):
                    P.dma("sp", wg[ws][:, k0:k0 + 2, :], wgv[e][:, k0:k0 + 2, :])
                P.dma("sp", wd[ws], wdv[e])
                for blk in range(NBP):
                    bs = (wi * NBP + blk) % 2
                    tsl = slice(blk * 512, (blk + 1) * 512)
                    for fc in range(8):
                        fs = fc % 2
                        pg = next_pmm(K)
                        for k in range(8):
                            P.call("pe", "matmul", out=pg, lhsT=wg[ws][:, k, fc * 128:(fc + 1) * 128], rhs=hT[:, k, tsl], start=(k == 0), stop=(k == 7))
                        pu = next_pmm(K)
                        for k in range(8):
                            P.call("pe", "matmul", out=pu, lhsT=wg[ws][:, k, D + fc * 128:D + (fc + 1) * 128], rhs=hT[:, k, tsl], start=(k == 0), stop=(k == 7))
                        P.call("dve", "tensor_scalar", out=gt[fs], in0=pg, scalar1=bgu[:, fc, e:e + 1], scalar2=7.0, op0=ALU.add, op1=ALU.min)
                        P.call("act", "activation", out=sg[fs], in_=gt[fs], func=AF.Sigmoid, scale=1.702)
                        P.call("dve", "tensor_scalar", out=ut[fs], in0=pu, scalar1=bgu[:, 8 + fc, e:e + 1], scalar2=7.0, op0=ALU.add, op1=ALU.min)
                        P.call("dve", "tensor_scalar", out=ut[fs], in0=ut[fs], scalar1=-7.0, scalar2=1.0, op0=ALU.max, op1=ALU.add)
                        P.call("pool", "tensor_tensor", out=gt[fs], in0=gt[fs], in1=sg[fs], op=ALU.mult)
                        P.call("pool", "tensor_tensor", out=actT[bs][:, fc, :], in0=gt[fs], in1=ut[fs], op=ALU.mult)
                    for jj in range(4):
                        j = blk * 4 + jj
                        i = t0 + j
                        pps = [K.pacc[0], K.pacc[1]]
                        for hlf in range(2):
                            for fc in range(8):
                                P.call("pe", "matmul", out=pps[hlf], lhsT=actT[bs][:, fc, jj * 128:(jj + 1) * 128], rhs=wd[ws][:, fc, hlf * 512:(hlf + 1) * 512], start=(fc == 0), stop=(fc == 7))
                            P.call("dve", "scalar_tensor_tensor", out=acc[:, j, hlf * 512:(hlf + 1) * 512], in0=pps[hlf], scalar=G[:, i, e:e + 1], in1=acc[:, j, hlf * 512:(hlf + 1) * 512], op0=ALU.mult, op1=ALU.add)
            for j in range(NTP):
                i = t0 + j
                s = i % 2
                P.dma("sp", xt[s], K.hown[i * 128:(i + 1) * 128, :])
                P.call("dve", "scalar_tensor_tensor", out=rt, in0=xt[s], scalar=ALPHA, in1=acc[:, j, :], op0=ALU.mult, op1=ALU.add)
                ln_tile(P, K, A, rt, ht, g3, b3, s)
                P.dma("sp", K.hnext[i * 128:(i + 1) * 128, :], ht)
        P.end_phase()
    swap_h(K)

import ml_dtypes


def host_consts(S, hf, b=0):
    TT = S // 2
    t = np.arange(S)
    row = (t // 64).astype(np.float32)
    col = (t % 64).astype(np.float32)

    def tabs(pos, dim):
        inv = (10000.0 ** (-np.arange(0, dim, 2, dtype=np.float32) / dim)).astype(np.float32)
        ang = pos[:, None] * inv[None, :]
        return np.cos(ang).astype(np.float32), np.sin(ang).astype(np.float32)
    cr, sr = tabs(row, 32)
    cc, sc = tabs(col, 32)
    cosg = np.concatenate([cr, cr, cc, cc], 1)
    sing = np.concatenate([-sr, sr, -sc, sc], 1)
    cr, sr = tabs(row, 16)
    cc, sc = tabs(col, 16)
    cosm = np.concatenate([cr, cr, cc, cc], 1)
    sinm = np.concatenate([-sr, sr, -sc, sc], 1)
    ropefull = np.ascontiguousarray(np.concatenate([cosg, sing, cosm, sinm], 1).astype(np.float32))
    ropeown = np.ascontiguousarray(ropefull[hf * TT:(hf + 1) * TT])
    m = np.arange(64)
    a64 = 2.0 * np.pi * ((m[:, None] * m[None, :]) % 64) / 64.0
    bc = np.kron(np.eye(4), np.cos(a64)).astype(np.float32)
    bsn = (-np.kron(np.eye(4), np.sin(a64))).astype(np.float32)
    cmask = np.zeros((128, 7), np.float32)
    cmask[:, 3 + b] = 1.0
    cmask[:, 0] = 1.0 if hf == 1 else 0.0
    cmask[:, 1] = 1.0 if hf == 0 else 0.0
    cmask[:, 2] = ((-1.0) ** np.arange(128)) if hf == 1 else 1.0
    return dict(ident=np.eye(128, dtype=np.float32), ropefull=ropefull, ropeown=ropeown,
                bc=bc, bsn=bsn, cmask=cmask)


def dft_shard(S, c):
    TT = S // 2
    n = (np.arange(S // 8, dtype=np.int64) + c * (S // 8))[:, None]
    k = np.arange(TT, dtype=np.int64)[None, :]
    ang = 2.0 * np.pi * ((n * k) % S).astype(np.float64) / S
    return np.cos(ang).astype(ml_dtypes.bfloat16), np.sin(ang).astype(ml_dtypes.bfloat16)


def make_in_maps(inputs, S, L, moe=True):
    TT = S // 2
    x = np.asarray(inputs["x"], np.float32)
    consts = {(b_, hf_): host_consts(S, hf_, b_) for b_ in range(4) for hf_ in range(2)}
    maps = []
    direct = ["ln_in_g", "ln_in_b", "w_in", "w_f", "b_f", "dw_w", "dw_b", "conv_ln_g", "conv_ln_b", "w_pw", "b_pw",
              "q_norm_g", "k_norm_g", "mla_q_norm_g", "w_uq", "mla_kv_norm_g", "w_ukv", "w_o",
              "ln1_g", "ln1_b", "ln2_g", "ln2_b", "ln3_g", "ln3_b", "w_xq", "w_xk", "w_xv", "w_xo",
              "w_router", "b_router", "b_gu", "b_down"]
    for c in range(8):
        b, hf = c // 2, c % 2
        m = {}
        m["xown"] = np.ascontiguousarray(x[b, hf * TT:(hf + 1) * TT])
        m["mem"] = np.ascontiguousarray(np.asarray(inputs["mem"], np.float32)[b])
        for n_ in direct:
            m[n_] = np.ascontiguousarray(np.asarray(inputs[n_], np.float32)[:L] if np.asarray(inputs[n_]).shape[0] == 2 and n_ not in ("ln_in_g", "ln_in_b") else np.asarray(inputs[n_], np.float32))
        m["grp_norm_g"] = np.ascontiguousarray(np.asarray(inputs["grp_norm_g"], np.float32)[:L].reshape(L, 1024))
        if moe:
            wg = np.asarray(inputs["w_gu"], np.float32)[:L, 4 * c:4 * c + 4]
            m["w_gu_sh"] = np.ascontiguousarray(wg.reshape(L, 4 * 1024, 2048))
            wd = np.asarray(inputs["w_down"], np.float32)[:L, 4 * c:4 * c + 4]
            m["w_down_sh"] = np.ascontiguousarray(wd.reshape(L, 4 * 1024, 1024))
        m.update(consts[(b, hf)])
        m["dftc_sh"], m["dfts_sh"] = dft_shard(S, c)
        maps.append(m)
    return maps


_PROG_CACHE = {}


def kernel(**inputs):
    S, L = 8192, 2
    cfg = dict(S=S, L=L)
    if "full" not in _PROG_CACHE:
        _PROG_CACHE["full"] = build_program(cfg)
    nc = _PROG_CACHE["full"]
    maps = make_in_maps(inputs, S, L)
    res = run_bass_kernel_spmd(nc, maps, core_ids=list(range(8)))
    TT = S // 2
    out = np.zeros((4, S, D), np.float32)
    for c in range(8):
        b, hf = c // 2, c % 2
        out[b, hf * TT:(hf + 1) * TT] = res.results[c]["out"]
    return out
```
